# Optimizing a Trainium2 kernel written in Bass

```python
import jax, jax.numpy as jnp
from jax import lax
import numpy as np

D_MODEL = 1024
BATCH = 2
SEQ = 16384
DEPTH = 2

MLSTM_HEADS = 4
MLSTM_QK_DIM = 64
MLSTM_V_DIM = 128
MLSTM_QK_W = MLSTM_HEADS * MLSTM_QK_DIM
MLSTM_W = MLSTM_HEADS * MLSTM_V_DIM
MLSTM_CHUNK = 128
QK_CONV = 4
FORGET_BIAS = 3.0
ATTN_HEADS = 8
ATTN_KV_HEADS = 2
ATTN_HEAD_DIM = 64
ATTN_W = ATTN_HEADS * ATTN_HEAD_DIM
ATTN_KV_W = ATTN_KV_HEADS * ATTN_HEAD_DIM
WINDOW = 128
ROPE_THETA = 10000.0
MIX_W = MLSTM_W + ATTN_W
SPLITS = (MLSTM_QK_W, MLSTM_QK_W, MLSTM_W, MLSTM_W, 2 * MLSTM_HEADS, ATTN_W, ATTN_KV_W, ATTN_KV_W)
IN_W = sum(SPLITS)
D_FF = 2816
FFN_CONV = 3
EPS = 1e-6

kernel_name = "hymba_mlstm_swa_sink_convffn"


def rmsnorm(x, g):
    xf = x.astype(jnp.float32)
    y = xf * lax.rsqrt(jnp.mean(xf * xf, axis=-1, keepdims=True) + EPS)
    return (y * g.astype(jnp.float32)).astype(x.dtype)


def causal_dwconv(x, w):
    K, C = w.shape
    return lax.conv_general_dilated(
        x, w[:, None, :].astype(x.dtype), window_strides=(1,), padding=[(K - 1, 0)],
        dimension_numbers=('NWC', 'WIO', 'NWC'), feature_group_count=C)


def rope(x):
    S, D = x.shape[1], x.shape[-1]
    inv = 1.0 / (ROPE_THETA ** (jnp.arange(0, D, 2, dtype=jnp.float32) / D))
    ang = jnp.arange(S, dtype=jnp.float32)[:, None] * inv[None, :]
    cos = jnp.cos(ang)[None, :, None, :]
    sin = jnp.sin(ang)[None, :, None, :]
    xf = x.astype(jnp.float32)
    x1, x2 = xf[..., :D // 2], xf[..., D // 2:]
    return jnp.concatenate([x1 * cos - x2 * sin, x2 * cos + x1 * sin], axis=-1)


def mlstm_chunkwise(q, k, v, log_i, log_f):
    B, S, H, DK = q.shape
    DV = v.shape[-1]
    L = MLSTM_CHUNK
    NC = S // L

    def chunks(a):
        a = a.reshape((B, NC, L) + a.shape[2:])
        return jnp.moveaxis(a, (1, 3), (0, 2))

    qc = chunks(q)
    kc = chunks(k * (DK ** -0.5))
    vc = chunks(v)
    lic = chunks(log_i)
    bc = jnp.cumsum(chunks(log_f), axis=-1)
    causal = jnp.tril(jnp.ones((L, L), dtype=bool))

    def step(carry, inp):
        C, n, m = carry
        q_, k_, v_, b_, li_ = inp
        dlog = b_[..., :, None] - b_[..., None, :] + li_[..., None, :]
        dlog = jnp.where(causal, dlog, -jnp.inf)
        inter = b_ + m[..., None]
        m_row = jnp.maximum(inter, jnp.max(dlog, axis=-1))
        w_intra = jnp.exp(dlog - m_row[..., None])
        w_inter = jnp.exp(inter - m_row)
        s = jnp.einsum('bhtd,bhsd->bhts', q_, k_) * w_intra
        num = (w_inter[..., None] * jnp.einsum('bhtd,bhde->bhte', q_, C)
               + jnp.einsum('bhts,bhse->bhte', s, v_))
        den = w_inter * jnp.einsum('bhtd,bhd->bht', q_, n) + jnp.sum(s, axis=-1)
        h = num / jnp.maximum(jnp.abs(den), jnp.exp(-m_row))[..., None]
        b_last = b_[..., -1]
        ls = b_last[..., None] - b_ + li_
        m_new = jnp.maximum(b_last + m, jnp.max(ls, axis=-1))
        decay = jnp.exp(b_last + m - m_new)
        ws = jnp.exp(ls - m_new[..., None])
        C = decay[..., None, None] * C + jnp.einsum('bhs,bhsd,bhse->bhde', ws, k_, v_)
        n = decay[..., None] * n + jnp.einsum('bhs,bhsd->bhd', ws, k_)
        return (C, n, m_new), h

    init = (jnp.zeros((B, H, DK, DV), jnp.float32), jnp.zeros((B, H, DK), jnp.float32),
            jnp.zeros((B, H), jnp.float32))
    _, hs = lax.scan(step, init, (qc, kc, vc, bc, lic))
    return jnp.moveaxis(hs, (0, 2), (1, 3)).reshape(B, S, H, DV)


def sliding_window_attention(q, k, v, sinks):
    B, S, H, D = q.shape
    KV = k.shape[2]
    G = H // KV
    W = WINDOW
    NB = S // W
    qb = q.reshape(B, NB, W, KV, G, D)
    kb = k.reshape(B, NB, W, KV, D)
    vb = v.reshape(B, NB, W, KV, D)
    kk = jnp.concatenate([jnp.concatenate([jnp.zeros_like(kb[:, :1]), kb[:, :-1]], axis=1), kb], axis=2)
    vv = jnp.concatenate([jnp.concatenate([jnp.zeros_like(vb[:, :1]), vb[:, :-1]], axis=1), vb], axis=2)
    s = jnp.einsum('bnqkgd,bnskd->bnkgqs', qb, kk) * (D ** -0.5)
    qpos = jnp.arange(W)[:, None] + W
    kpos = jnp.arange(2 * W)[None, :]
    diff = qpos - kpos
    band = (diff >= 0) & (diff < W)
    first = (jnp.arange(NB) == 0)[:, None, None]
    valid = band[None] & ~(first & (kpos[None] < W))
    s = jnp.where(valid[None, :, None, None], s, -jnp.inf)
    sink = sinks.astype(jnp.float32).reshape(1, 1, KV, G, 1, 1)
    mx = jnp.maximum(jnp.max(s, axis=-1, keepdims=True), sink)
    p = jnp.exp(s - mx)
    denom = jnp.sum(p, axis=-1, keepdims=True) + jnp.exp(sink - mx)
    o = jnp.einsum('bnkgqs,bnskd->bnqkgd', p / denom, vv)
    return o.reshape(B, S, H * D)


def token_mixer(h, w_in, qk_conv_w, qk_conv_b, gate_bias, mh_norm_g, attn_sinks, w_out):
    B, S, _ = h.shape
    z = h @ w_in
    parts = []
    off = 0
    for w in SPLITS:
        parts.append(z[..., off:off + w])
        off += w
    mq, mk, mv, mo, mif, aq, ak, av = parts
    qk = jax.nn.silu(causal_dwconv(jnp.concatenate([mq, mk], axis=-1), qk_conv_w) + qk_conv_b)
    qk = qk.astype(jnp.float32)
    mq_h = qk[..., :MLSTM_QK_W].reshape(B, S, MLSTM_HEADS, MLSTM_QK_DIM)
    mk_h = qk[..., MLSTM_QK_W:].reshape(B, S, MLSTM_HEADS, MLSTM_QK_DIM)
    mv_h = mv.astype(jnp.float32).reshape(B, S, MLSTM_HEADS, MLSTM_V_DIM)
    gates = mif.astype(jnp.float32) + gate_bias.astype(jnp.float32)
    log_i = gates[..., :MLSTM_HEADS]
    log_f = jax.nn.log_sigmoid(gates[..., MLSTM_HEADS:])
    ht = mlstm_chunkwise(mq_h, mk_h, mv_h, log_i, log_f)
    ht = ht * lax.rsqrt(jnp.mean(ht * ht, axis=-1, keepdims=True) + EPS)
    m_out = ht.reshape(B, S, MLSTM_W) * mh_norm_g.astype(jnp.float32) * jax.nn.sigmoid(mo.astype(jnp.float32))
    q = rope(aq.reshape(B, S, ATTN_HEADS, ATTN_HEAD_DIM))
    k = rope(ak.reshape(B, S, ATTN_KV_HEADS, ATTN_HEAD_DIM))
    v = av.astype(jnp.float32).reshape(B, S, ATTN_KV_HEADS, ATTN_HEAD_DIM)
    a_out = sliding_window_attention(q, k, v, attn_sinks)
    y = jnp.concatenate([m_out, a_out], axis=-1).astype(h.dtype)
    return y @ w_out


def conv_ffn(h, w_up, ffn_conv_w, w_down):
    u = causal_dwconv(h @ w_up, ffn_conv_w)
    gate, val = u[..., :D_FF], u[..., D_FF:]
    return (jax.nn.silu(gate) * val) @ w_down


def setup_inputs(seed: int = 0) -> dict:
    key = jax.random.key(seed)
    ks = jax.random.split(key, 20)
    f32 = jnp.float32
    nrm = lambda k, shape: jax.random.normal(k, shape, f32)
    return {
        "x": nrm(ks[0], (BATCH, SEQ, D_MODEL)),
        "g_pre_mix": 1.0 + 0.05 * nrm(ks[1], (DEPTH, D_MODEL)),
        "w_in": nrm(ks[2], (DEPTH, D_MODEL, IN_W)) * D_MODEL ** -0.5,
        "qk_conv_w": nrm(ks[3], (DEPTH, QK_CONV, 2 * MLSTM_QK_W)) * QK_CONV ** -0.5,
        "qk_conv_b": 0.02 * nrm(ks[4], (DEPTH, 2 * MLSTM_QK_W)),
        "gate_bias": jnp.concatenate([0.1 * nrm(ks[5], (DEPTH, MLSTM_HEADS)),
                                      FORGET_BIAS + 0.5 * nrm(ks[6], (DEPTH, MLSTM_HEADS))], axis=-1),
        "mh_norm_g": 1.0 + 0.05 * nrm(ks[7], (DEPTH, MLSTM_W)),
        "attn_sinks": 0.5 * nrm(ks[8], (DEPTH, ATTN_HEADS)),
        "w_out": nrm(ks[9], (DEPTH, MIX_W, D_MODEL)) * MIX_W ** -0.5,
        "g_post_mix": 1.0 + 0.05 * nrm(ks[10], (DEPTH, D_MODEL)),
        "g_pre_ffn": 1.0 + 0.05 * nrm(ks[11], (DEPTH, D_MODEL)),
        "w_up": nrm(ks[12], (DEPTH, D_MODEL, 2 * D_FF)) * D_MODEL ** -0.5,
        "ffn_conv_w": nrm(ks[13], (DEPTH, FFN_CONV, 2 * D_FF)) * FFN_CONV ** -0.5,
        "w_down": nrm(ks[14], (DEPTH, D_FF, D_MODEL)) * D_FF ** -0.5,
        "g_post_ffn": 1.0 + 0.05 * nrm(ks[15], (DEPTH, D_MODEL)),
    }


def reference(x, g_pre_mix, w_in, qk_conv_w, qk_conv_b, gate_bias, mh_norm_g, attn_sinks, w_out,
              g_post_mix, g_pre_ffn, w_up, ffn_conv_w, w_down, g_post_ffn):
    for l in range(DEPTH):
        h = rmsnorm(x, g_pre_mix[l])
        y = token_mixer(h, w_in[l], qk_conv_w[l], qk_conv_b[l], gate_bias[l], mh_norm_g[l],
                        attn_sinks[l], w_out[l])
        x = x + rmsnorm(y, g_post_mix[l])
        h = rmsnorm(x, g_pre_ffn[l])
        y = conv_ffn(h, w_up[l], ffn_conv_w[l], w_down[l])
        x = x + rmsnorm(y, g_post_ffn[l])
    return x
```

```python
import numpy as np
from contextlib import ExitStack
import concourse.bass as bass
import concourse.mybir as mybir
from concourse.bass_utils import run_bass_kernel_spmd

F32 = mybir.dt.float32
BF16 = mybir.dt.bfloat16
AF = mybir.ActivationFunctionType
ALU = mybir.AluOpType

NCORES = 8
D = 1024
SEG = 4096
T = 512
NT = SEG // T
HAL = 128
L = 2
INW = 2312
DFF = 2816
EPS = 1e-6
NEG = -30000.0
LN8 = float(np.log(0.125))
PL = 712
NCST = 128 * 3 + 512 * 3 + 16
P_G, P_QW, P_QB, P_FW, P_GMH, P_GB, P_SK = 0, 32, 48, 52, 184, 696, 704
O_QK, O_V, O_O, O_AQ, O_AKV, O_GT = 0, 4096, 8192, 12288, 16384, 18432
O_OUT = 18496
O_UP = O_OUT + 2 * 4096
O_DN = O_UP + 11 * 4096
WTOT = O_DN + 8 * 22 * 128


class Buf:
    def __init__(self, name):
        self.name = name
        self.w = None
        self.r = {}
        self.sem = None
        self.cnt = 0
        self.excl = False


class Eng:
    def __init__(self, name, e, pe=False):
        self.name = name
        self.e = e
        self.pe = pe
        self.sem = None
        self.cnt = 0
        self.seen = {}


class KB:
    def __init__(self, nc):
        self.nc = nc
        self.es = ExitStack()
        self.nsem = 0
        self.pe = self.mk("pe", nc.tensor, True)
        self.act = self.mk("act", nc.scalar)
        self.dve = self.mk("dve", nc.vector)
        self.pool = self.mk("pool", nc.gpsimd)
        self.sp = self.mk("sp", nc.sync)
        self.allbufs = []

    def newsem(self, name):
        self.nsem += 1
        return self.es.enter_context(self.nc.semaphore(f"{name}_{self.nsem}"))

    def mk(self, name, e, pe=False):
        g = Eng(name, e, pe)
        g.sem = self.newsem(name)
        return g

    def buf(self, name):
        b = Buf(name)
        self.allbufs.append(b)
        return b

    def sb(self, name, shape, dt=F32):
        t = self.es.enter_context(self.nc.sbuf_tensor("s_" + name, list(shape), dt))
        return t, self.buf(name)

    def ps(self, name, shape, dt=F32):
        t = self.es.enter_context(self.nc.psum_tensor("p_" + name, list(shape), dt))
        b = self.buf(name)
        b.excl = True
        return t, b

    def wait(self, eng, ev):
        sem, val, src, key = ev
        if src is eng and eng.pe:
            return
        if eng.seen.get(key, 0) >= val:
            return
        eng.e.wait_ge(sem, val)
        eng.seen[key] = val

    def deps(self, eng, rd, wr):
        for b in rd:
            if b.w is not None:
                self.wait(eng, b.w)
            if b.excl:
                for ev in b.r.values():
                    if ev[2] is not eng:
                        self.wait(eng, ev)
        for b in wr:
            if b.w is not None:
                self.wait(eng, b.w)
            for ev in b.r.values():
                self.wait(eng, ev)

    def mark(self, ev, rd, wr, rkey):
        for b in rd:
            b.r[rkey] = ev
        for b in wr:
            b.w = ev
            b.r = {}

    def op(self, eng, fn, rd=(), wr=()):
        self.deps(eng, rd, wr)
        ins = fn(eng.e)
        eng.cnt += 1
        ins.then_inc(eng.sem, 1)
        ev = (eng.sem, eng.cnt, eng, eng.name)
        self.mark(ev, rd, wr, eng.name)

    def dma(self, eng, out, in_, rd=(), wr=(), sb=None):
        self.deps(eng, rd, wr)
        ins = eng.e.dma_start(out=out, in_=in_)
        if sb.sem is None:
            sb.sem = self.newsem("d")
        sb.cnt += 16
        ins.then_inc(sb.sem, 16)
        ev = (sb.sem, sb.cnt, None, "dma_" + sb.name)
        self.mark(ev, rd, wr, "dma_" + sb.name + str(sb.cnt))

    def coll(self, ins_ap, outs_ap, rd, wr, sb):
        eng = self.pool
        self.deps(eng, rd, wr)
        ins = eng.e.collective_compute("AllGather", ALU.bypass, replica_groups=[list(range(NCORES))],
                                       ins=[ins_ap], outs=[outs_ap])
        csem = self.newsem("c")
        ins.then_inc(csem)
        eng.e.wait_ge(csem, 1)
        ev = (csem, 1, None, "cc_%d" % self.nsem)
        self.mark(ev, rd, wr, "cc_%d" % self.nsem)

    def finish(self, bufs):
        for b in bufs:
            if b.w is not None:
                self.wait(self.sp, b.w)


def build(phase):
    nc = bass.Bass("TRN2", target_bir_lowering=False)
    kb = KB(nc)
    PE, ACT, DVE, POOL, SP = kb.pe, kb.act, kb.dve, kb.pool, kb.sp

    def din(name, shape):
        return nc.dram_tensor(name, list(shape), F32, kind="ExternalInput").ap()

    xT_in = din("xT", [128, 8 * SEG])
    xh_in = din("xh", [128, 8 * HAL])
    prm_in = din("prm", [128, PL])
    cst_in = din("cst", [128, NCST])
    SW = 2 * 129 + 4
    if phase in ("A", "C"):
        w_in = din("w_in", [D, INW])
    if phase == "C":
        w_out = din("w_out", [D, D])
        rope_in = din("rope", [128, 2 * (HAL + SEG)])
        sgall = din("sgall", [NCORES * 128, SW])
    if phase == "D":
        w_up = din("w_up", [D, 2 * DFF])
        w_dn = din("w_down", [DFF, D])
    if phase == "A":
        spk_out = nc.dram_tensor("spk", [128, SW], F32, kind="ExternalOutput").ap()
        b_spko = kb.buf("spko")
    else:
        xo_d = nc.dram_tensor("xo", [128, 8 * SEG], F32, kind="ExternalOutput").ap()
    wbf = [nc.dram_tensor("wbf0", [128, WTOT], BF16).ap()]
    XW = 8 * HAL
    b_xo = [kb.buf(f"xo{i}") for i in range(NT)]
    b_in = kb.buf("inputs")

    prm, b_prm = kb.sb("prm", [128, PL])
    prmh, b_prmh = kb.sb("prmh", [128, PL])
    cst, b_cst = kb.sb("cst", [128, 400])
    ident_bf, b_ident = kb.sb("ident_bf", [128, 128], BF16)
    pswap_bf, b_pswap = kb.sb("pswap_bf", [128, 128], BF16)
    mb_bf, b_mb = kb.sb("mb_bf", [128, 3, 512], BF16)
    onesn_bf, b_onesn = kb.sb("onesn_bf", [128, 128], BF16)
    ones_f, b_onesf = kb.sb("ones_f", [128, 128])
    esink, b_esink = kb.sb("esink", [128, 8])
    C_ID, C_TRI, C_SW, C_SEL = 0, 128, 256, 384
    tri_f = cst[:, C_TRI:C_TRI + 128]

    x_t = [kb.sb(f"x_t{i}", [128, 8, T]) for i in range(1)]
    sq_t, b_sq = kb.sb("sq_t", [128, 8, T], BF16)
    rstd_t, b_rstd = kb.sb("rstd_t", [128, T])
    hT, b_hT = kb.sb("hT", [128, 8, T], BF16)
    ybuf, b_ybuf = kb.sb("ybuf", [128, 8, T])
    yT, b_yT = kb.sb("yT_sb", [128, 8, T], BF16)
    wring = [kb.sb(f"wr{i}", [128, 4096], BF16) for i in range(3)]
    wri = [0]
    rope_t = [kb.sb(f"rope{i}", [128, 2, T]) for i in range(1)]
    qkpre, b_qkpre = kb.sb("qkpre", [128, 4, 3 + T])
    cacc, b_cacc = kb.sb("cacc", [128, T])
    ctanh, b_ctanh = kb.sb("ctanh", [128, T])
    qkT, b_qkT = kb.sb("qkT", [128, 4, T], BF16)
    v_aug, b_vaug = kb.sb("v_aug", [128, 4, 4, 129], BF16)
    sig_o, b_sigo = kb.sb("sig_o", [128, 512])
    aqT, b_aqT = kb.sb("aqT", [128, 4, T], BF16)
    akz, b_akT = kb.sb("akz", [128, 2, HAL + T], BF16)
    av_ext, b_av = kb.sb("av_ext", [128, 5, 130], BF16)
    a_bf, b_abf = kb.sb("a_bf", [128, T], BF16)
    r1, b_r1 = kb.sb("r1", [128, T])
    r2, b_r2 = kb.sb("r2", [128, T])
    gp, b_gp = kb.sb("gp", [128, 4, 8])
    ge, b_ge = kb.sb("ge", [128, 4, 4])
    lf, b_lf = kb.sb("lf", [128, 4, 4])
    gsm, b_gsm = kb.sb("gsm", [128, 6, 16])
    lfrep, b_lfrep = kb.sb("lfrep", [128, 4, 128])
    Dt, b_Dt = kb.sb("Dt", [128, 512])
    eb, b_eb = kb.sb("eb", [128, 512])
    SdT, b_SdT = kb.sb("SdT", [128, 512], BF16)
    qp, b_qp = kb.sb("qpz", [128, 4, 128], BF16)
    qz, b_qz = kb.sb("qz", [128, 4, 128], BF16)
    kpp, b_kpp = kb.sb("kpp", [128, 2, 128], BF16)
    Cst, b_Cst = kb.sb("Cst", [128, 2, 129])
    Cbf, b_Cbf = kb.sb("Cbf", [128, 2, 129], BF16)
    hh, b_hh = kb.sb("hh", [128, 4, 128])
    hjunk, b_hjunk = kb.sb("hjunk", [128, 128], BF16)
    hsm, b_hsm = kb.sb("hsm", [128, 8, 8])
    gs, b_gs = kb.sb("gs", [128, 512])
    ym, b_ym = kb.sb("ym", [128, 512], BF16)
    PT = [kb.sb(f"PT{i}", [128, 512], BF16) for i in range(4)]
    ya, b_ya = kb.sb("ya", [128, 512], BF16)
    g_t, b_g = kb.sb("g_t", [128, 22, T], BF16)
    upre = [kb.sb(f"upre{i}", [128, 2 + T]) for i in range(2)]
    uacc = [kb.sb(f"uacc{i}", [128, T]) for i in range(2)]
    utanh, b_utanh = kb.sb("utanh", [128, T])
    uu, b_uu = kb.sb("uu", [128, T])
    tails, b_tails = kb.sb("tails", [128, 44, 2])
    xg, b_xg = kb.sb("xg", [128, XW])
    xhalo, b_xhalo = kb.sb("xhalo", [128, 8, HAL])
    sg, b_sg = kb.sb("sg", [128, SW])
    spk, b_spk = kb.sb("spk", [128, SW])
    stmp, b_stmp = kb.sb("stmp", [128, SW])
    Btot, b_Btot = kb.sb("Btot", [128, 4])
    psf = [kb.ps(f"psf{i}", [128, 512]) for i in range(6)]
    psb = [kb.ps(f"psb{i}", [128, 1024], BF16) for i in range(2)]
    pi = [0, 0]

    def nps():
        pi[0] = (pi[0] + 1) % 6
        return psf[pi[0]]

    def npb():
        pi[1] = (pi[1] + 1) % 2
        return psb[pi[1]]

    kb.dma(SP, prm[:], prm_in, wr=[b_prm], sb=b_prm)
    kb.dma(SP, cst[:, 0:384], cst_in[:, 0:384], wr=[b_cst], sb=b_cst)
    kb.dma(SP, cst[:, 384:400], cst_in[:, 1920:1936], wr=[b_cst], sb=b_cst)
    kb.dma(POOL, mb_bf[:].rearrange("p a b -> p (a b)"), cst_in[:, 384:1920], wr=[b_mb], sb=b_mb)
    kb.op(DVE, lambda e: e.tensor_scalar(out=prmh[:], in0=prm[:], scalar1=0.5, scalar2=None, op0=ALU.mult),
          rd=[b_prm], wr=[b_prmh])
    kb.op(DVE, lambda e: e.tensor_copy(out=ident_bf[:], in_=cst[:, C_ID:C_ID + 128]), rd=[b_cst], wr=[b_ident])
    kb.op(DVE, lambda e: e.tensor_copy(out=pswap_bf[:], in_=cst[:, C_SW:C_SW + 128]), rd=[b_cst], wr=[b_pswap])
    kb.op(POOL, lambda e: e.memset(onesn_bf[:], 1.0 / 1024.0), wr=[b_onesn])
    kb.op(POOL, lambda e: e.memset(ones_f[:], 1.0), wr=[b_onesf])
    for l in range(1):
        kb.op(ACT, lambda e, l=l: e.activation(out=esink[:, l * 8:(l + 1) * 8],
                                               in_=prm[:, l * PL + P_SK:l * PL + P_SK + 8], func=AF.Exp),
              rd=[b_prm], wr=[b_esink])

    wblk = [dict() for _ in range(L)]
    pend = []

    def prep(l, key, dst_off, ncols_tot, pieces):
        b = kb.buf(f"wb{l}_{key}")
        wblk[l][key] = (b, dst_off, ncols_tot)
        for (dst, src) in pieces:
            if len(pend) >= 2:
                kb.wait(POOL, pend.pop(0))
            kb.dma(POOL, dst, src, wr=[b], sb=b)
            pend.append(b.w)

    def prep_layer(l):
        W = wbf[l]

        def blk(off, kc, n):
            return W[:, off:off + kc * n].rearrange("p (kc n) -> p kc n", kc=kc)
        if phase in ("A", "C"):
            wi = w_in.rearrange("(kc p) n -> p kc n", p=128)
            prep(l, "qk", O_QK, 4096, [(blk(O_QK, 8, 512), wi[:, :, 0:512])])
            prep(l, "v", O_V, 4096, [(blk(O_V, 8, 512), wi[:, :, 512:1024])])
            prep(l, "gt", O_GT, 64, [(blk(O_GT, 8, 8), wi[:, :, 1536:1544])])
        if phase == "C":
            wo = w_out.rearrange("(kc p) n -> p kc n", p=128)
            prep(l, "o", O_O, 4096, [(blk(O_O, 8, 512), wi[:, :, 1024:1536])])
            aq = W[:, O_AQ:O_AQ + 4096].rearrange("p (kc c j d) -> p kc c j d", kc=8, c=4, j=2)
            pcs = []
            for j in range(2):
                for kc in range(8):
                    pcs.append((aq[:, kc, :, j, :],
                                wi[:, kc, 1544 + j * 256:1544 + (j + 1) * 256].rearrange("p (c d) -> p c d", c=4)))
            prep(l, "aq", O_AQ, 4096, pcs)
            prep(l, "akv", O_AKV, 2048, [(blk(O_AKV, 8, 256), wi[:, :, 2056:2312])])
            for nb in range(2):
                prep(l, f"out{nb}", O_OUT + nb * 4096, 4096,
                     [(blk(O_OUT + nb * 4096, 8, 512), wo[:, :, nb * 512:(nb + 1) * 512])])
        if phase == "D":
            wu = w_up.rearrange("(kc p) n -> p kc n", p=128)
            wd = w_dn.rearrange("(j p) n -> p j n", p=128)
            for b in range(11):
                d = blk(O_UP + b * 4096, 8, 512)
                prep(l, f"up{b}", O_UP + b * 4096, 4096,
                     [(d[:, :, 0:256], wu[:, :, b * 256:(b + 1) * 256]),
                      (d[:, :, 256:512], wu[:, :, DFF + b * 256:DFF + (b + 1) * 256])])
            for m in range(8):
                prep(l, f"dn{m}", O_DN + m * 2816, 2816,
                     [(blk(O_DN + m * 2816, 22, 128), wd[:, :, m * 128:(m + 1) * 128])])

    prep_layer(0)

    def wload(l, key):
        b, off, n = wblk[l][key]
        wri[0] = (wri[0] + 1) % 3
        t, tb = wring[wri[0]]
        kb.dma(SP, t[:, 0:n], wbf[l][:, off:off + n], rd=[b], wr=[tb], sb=tb)
        return t, tb

    def load_x(src_ap, src_buf, i, slot, n=T, col0=None):
        xt, xb = x_t[slot]
        c0 = i * T if col0 is None else col0
        kb.dma(SP, xt[:, :, 0:n], src_ap.rearrange("p (c t) -> p c t", c=8)[:, :, c0:c0 + n],
               rd=[src_buf], wr=[xb], sb=xb)
        return xt, xb

    def norm_stats(src, bsrc, n):
        kb.op(ACT, lambda e: e.activation(out=sq_t[:, :, 0:n], in_=src[:, :, 0:n], func=AF.Square),
              rd=[bsrc], wr=[b_sq])
        ps, bp = nps()

        def f(e):
            for c in range(8):
                r = e.matmul(ps[:, 0:n], lhsT=onesn_bf[:], rhs=sq_t[:, c, 0:n], start=(c == 0), stop=(c == 7))
            return r
        kb.op(PE, f, rd=[b_sq, b_onesn], wr=[bp])
        kb.op(ACT, lambda e: e.activation(out=rstd_t[:, 0:n], in_=ps[:, 0:n], func=AF.Sqrt, bias=EPS),
              rd=[bp], wr=[b_rstd])
        kb.op(DVE, lambda e: e.reciprocal(out=rstd_t[:, 0:n], in_=rstd_t[:, 0:n]), rd=[], wr=[b_rstd])

    def prenorm(xt, xb, l, nidx, n):
        norm_stats(xt, xb, n)
        for c in range(8):
            gc = prm[:, l * PL + P_G + nidx * 8 + c:l * PL + P_G + nidx * 8 + c + 1]
            kb.op(DVE, lambda e, c=c, gc=gc: e.scalar_tensor_tensor(
                out=hT[:, c, 0:n], in0=xt[:, c, 0:n], scalar=gc, in1=rstd_t[:, 0:n],
                op0=ALU.mult, op1=ALU.mult), rd=[xb, b_rstd, b_prm], wr=[b_hT])

    def postnorm_add(xt, xb, l, nidx, n):
        norm_stats(ybuf, b_ybuf, n)
        for c in range(8):
            gc = prm[:, l * PL + P_G + nidx * 8 + c:l * PL + P_G + nidx * 8 + c + 1]
            kb.op(DVE, lambda e, c=c, gc=gc: e.scalar_tensor_tensor(
                out=ybuf[:, c, 0:n], in0=ybuf[:, c, 0:n], scalar=gc, in1=rstd_t[:, 0:n],
                op0=ALU.mult, op1=ALU.mult), rd=[b_rstd, b_prm], wr=[b_ybuf])
            kb.op(POOL, lambda e, c=c: e.tensor_tensor(out=xt[:, c, 0:n], in0=xt[:, c, 0:n], in1=ybuf[:, c, 0:n],
                                                       op=ALU.add), rd=[b_ybuf], wr=[xb])

    def mm_fm(ps, bp, w, bw, col0, n, kcn=8, rhs=None, brhs=None, wstride=None):
        rhs = hT if rhs is None else rhs
        brhs = b_hT if brhs is None else brhs

        def f(e):
            for kc in range(kcn):
                r = e.matmul(ps[:, 0:n], lhsT=w[:, kc * wstride + col0:kc * wstride + col0 + 128],
                             rhs=rhs[:, kc, 0:n], start=(kc == 0), stop=(kc == kcn - 1))
            return r
        kb.op(PE, f, rd=[bw, brhs], wr=[bp])

    def mm_tm(ps, bp, w, bw, col0, ncols, tok0, wstride):
        def f(e):
            for kc in range(8):
                r = e.matmul(ps[:, 0:ncols], lhsT=hT[:, kc, tok0:tok0 + 128],
                             rhs=w[:, kc * wstride + col0:kc * wstride + col0 + ncols],
                             start=(kc == 0), stop=(kc == 7))
            return r
        kb.op(PE, f, rd=[bw, b_hT], wr=[bp])

    def qk_conv(l, chunks, n):
        for ch in chunks:
            wc = lambda tap: prmh[:, l * PL + P_QW + ch * 4 + tap:l * PL + P_QW + ch * 4 + tap + 1]
            bc = prmh[:, l * PL + P_QB + ch:l * PL + P_QB + ch + 1]
            kb.op(DVE, lambda e: e.tensor_scalar(out=cacc[:, 0:n], in0=qkpre[:, ch, 3:3 + n], scalar1=wc(3), scalar2=bc,
                                                 op0=ALU.mult, op1=ALU.add), rd=[b_qkpre, b_prmh], wr=[b_cacc])
            for tap in range(3):
                kb.op(DVE, lambda e, tap=tap: e.scalar_tensor_tensor(
                    out=cacc[:, 0:n], in0=qkpre[:, ch, tap:tap + n], scalar=wc(tap), in1=cacc[:, 0:n],
                    op0=ALU.mult, op1=ALU.add), rd=[b_qkpre, b_prmh], wr=[b_cacc])
            kb.op(ACT, lambda e: e.activation(out=ctanh[:, 0:n], in_=cacc[:, 0:n], func=AF.Tanh), rd=[b_cacc], wr=[b_ctanh])
            kb.op(DVE, lambda e: e.scalar_tensor_tensor(out=qkT[:, ch, 0:n], in0=ctanh[:, 0:n], scalar=1.0, in1=cacc[:, 0:n],
                                                        op0=ALU.add, op1=ALU.mult), rd=[b_ctanh, b_cacc], wr=[b_qkT])
            kb.op(POOL, lambda e: e.tensor_copy(out=qkpre[:, ch, 0:3], in_=qkpre[:, ch, n:n + 3]), rd=[], wr=[b_qkpre])

    def gates(l, w, bw, nch):
        for c in range(nch):
            ps, bp = nps()
            mm_tm(ps, bp, w, bw, 0, 8, c * 128, 8)
            kb.op(DVE, lambda e, c=c, ps=ps: e.tensor_tensor(out=gp[:, c, :], in0=ps[:, 0:8],
                                                            in1=prm[:, l * PL + P_GB:l * PL + P_GB + 8], op=ALU.add),
                  rd=[bp, b_prm], wr=[b_gp])
        kb.op(ACT, lambda e: e.activation(out=ge[:, 0:nch, :], in_=gp[:, 0:nch, 4:8], func=AF.Exp, scale=-1.0),
              rd=[b_gp], wr=[b_ge])
        kb.op(ACT, lambda e: e.activation(out=ge[:, 0:nch, :], in_=ge[:, 0:nch, :], func=AF.Ln, bias=1.0),
              rd=[], wr=[b_ge])
        kb.op(DVE, lambda e: e.tensor_scalar(out=lf[:, 0:nch, :], in0=ge[:, 0:nch, :], scalar1=-1.0, scalar2=None,
                                             op0=ALU.mult), rd=[b_ge], wr=[b_lf])
        lf2 = lf[:, 0:nch, :].rearrange("p a b -> p (a b)")
        nn = nch * 4
        ps, bp = nps()

        def f(e):
            e.matmul(ps[:, 0:nn], lhsT=tri_f, rhs=lf2, start=True, stop=True)
            return e.matmul(ps[:, 16:16 + nn], lhsT=ones_f[:], rhs=lf2, start=True, stop=True)
        kb.op(PE, f, rd=[b_cst, b_lf, b_onesf], wr=[bp])
        li = gp[:, 0:nch, 0:4]
        g3 = lambda r: gsm[:, r, 0:nn].rearrange("p (a b) -> p a b", a=nch)
        kb.op(DVE, lambda e: e.tensor_tensor(out=g3(4), in0=li, in1=ps[:, 0:nn].rearrange("p (a b) -> p a b", a=nch),
                                             op=ALU.subtract), rd=[b_gp, bp], wr=[b_gsm])
        kb.op(DVE, lambda e: e.tensor_scalar(out=gsm[:, 0, 0:nn], in0=gsm[:, 4, 0:nn], scalar1=LN8, scalar2=None, op0=ALU.add),
              rd=[], wr=[b_gsm])
        kb.op(DVE, lambda e: e.tensor_tensor(out=gsm[:, 1, 0:nn], in0=gsm[:, 0, 0:nn], in1=ps[:, 16:16 + nn], op=ALU.add),
              rd=[bp], wr=[b_gsm])
        kb.op(ACT, lambda e: e.activation(out=gsm[:, 2, 0:nn], in_=gsm[:, 1, 0:nn], func=AF.Exp), rd=[], wr=[b_gsm])
        kb.op(ACT, lambda e: e.activation(out=gsm[:, 3, 0:nn], in_=ps[:, 16:16 + nn], func=AF.Exp), rd=[bp], wr=[b_gsm])
        for c in range(nch):
            kb.op(DVE, lambda e, c=c: e.tensor_tensor(out=Btot[:], in0=Btot[:], in1=ps[:, 16 + c * 4:16 + c * 4 + 4], op=ALU.add),
                  rd=[bp], wr=[b_Btot])

    def state_update(c):
        pb, bpb = npb()

        def f(e):
            e.transpose(pb[:, 0:128], qkT[:, 2, c * 128:(c + 1) * 128], ident_bf[:])
            return e.transpose(pb[:, 128:256], qkT[:, 3, c * 128:(c + 1) * 128], ident_bf[:])
        kb.op(PE, f, rd=[b_qkT, b_ident], wr=[bpb])
        for cc in range(2):
            for j in range(2):
                h = 2 * cc + j
                kb.op(DVE, lambda e, cc=cc, j=j, h=h: e.tensor_scalar(
                    out=kpp[:, cc, j * 64:(j + 1) * 64], in0=pb[:, cc * 128 + j * 64:cc * 128 + (j + 1) * 64],
                    scalar1=gsm[:, 2, c * 4 + h:c * 4 + h + 1], scalar2=None, op0=ALU.mult),
                    rd=[bpb, b_gsm], wr=[b_kpp])
        for cc in range(2):
            ps, bp = nps()

            def f2(e, cc=cc, ps=ps):
                for j in range(2):
                    r = e.matmul(ps[:, j * 129:(j + 1) * 129], lhsT=kpp[:, cc, :], rhs=v_aug[:, c, 2 * cc + j, :],
                                 start=True, stop=True)
                return r
            kb.op(PE, f2, rd=[b_kpp, b_vaug], wr=[bp])
            for j in range(2):
                h = 2 * cc + j
                sl = slice(j * 64, (j + 1) * 64)
                kb.op(DVE, lambda e, cc=cc, j=j, h=h, sl=sl, ps=ps: e.scalar_tensor_tensor(
                    out=Cst[sl, cc, :], in0=Cst[sl, cc, :], scalar=gsm[sl, 3, c * 4 + h:c * 4 + h + 1],
                    in1=ps[sl, j * 129:(j + 1) * 129], op0=ALU.mult, op1=ALU.add),
                    rd=[bp, b_gsm], wr=[b_Cst])
        kb.op(ACT, lambda e: e.copy(out=Cbf[:], in_=Cst[:]), rd=[b_Cst], wr=[b_Cbf])

    def store_state():
        kb.op(DVE, lambda e: e.tensor_copy(out=spk[:, 0:258], in_=Cst[:].rearrange("p a b -> p (a b)")), rd=[b_Cst], wr=[b_spk])
        kb.op(DVE, lambda e: e.tensor_copy(out=spk[:, 258:262], in_=Btot[:]), rd=[b_Btot], wr=[b_spk])
        kb.dma(SP, spk_out, spk[:], rd=[b_spk], wr=[b_spko], sb=b_spko)

    def combine_state():
        kb.op(POOL, lambda e: e.memset(Cst[:], 0.0), wr=[b_Cst])
        for r in range(NCORES):
            kb.dma(SP, sg[:], sgall[r * 128:(r + 1) * 128, :], rd=[b_in], wr=[b_sg], sb=b_sg)
            a = cst[:, C_SEL + 8 + r:C_SEL + 8 + r + 1]
            kb.op(ACT, lambda e, r=r: e.activation(out=stmp[:, 258:262], in_=sg[:, 258:262], func=AF.Exp), rd=[b_sg], wr=[b_stmp])
            kb.op(DVE, lambda e: e.tensor_scalar(out=stmp[:, 258:262], in0=stmp[:, 258:262], scalar1=-1.0, scalar2=None,
                                                 op0=ALU.add), rd=[], wr=[b_stmp])
            kb.op(DVE, lambda e: e.tensor_scalar(out=stmp[:, 258:262], in0=stmp[:, 258:262], scalar1=a, scalar2=None,
                                                 op0=ALU.mult), rd=[b_cst], wr=[b_stmp])
            kb.op(DVE, lambda e: e.tensor_scalar(out=stmp[:, 258:262], in0=stmp[:, 258:262], scalar1=1.0, scalar2=None,
                                                 op0=ALU.add), rd=[], wr=[b_stmp])
            kb.op(DVE, lambda e, r=r: e.tensor_scalar(out=stmp[:, 0:258], in0=sg[:, 0:258], scalar1=a, scalar2=None,
                                                      op0=ALU.mult), rd=[b_sg, b_cst], wr=[b_stmp])
            for cc in range(2):
                for j in range(2):
                    h = 2 * cc + j
                    sl = slice(j * 64, (j + 1) * 64)
                    kb.op(DVE, lambda e, cc=cc, sl=sl, h=h: e.scalar_tensor_tensor(
                        out=Cst[sl, cc, :], in0=Cst[sl, cc, :], scalar=stmp[sl, 258 + h:259 + h],
                        in1=stmp[sl, cc * 129:(cc + 1) * 129], op0=ALU.mult, op1=ALU.add), rd=[b_stmp], wr=[b_Cst])
        kb.op(ACT, lambda e: e.copy(out=Cbf[:], in_=Cst[:]), rd=[b_Cst], wr=[b_Cbf])

    def front_k(l, n, w_qk, bw_qk):
        for ch in (2, 3):
            ps, bp = nps()
            mm_fm(ps, bp, w_qk, bw_qk, ch * 128, n, wstride=512)
            kb.op(ACT, lambda e, ch=ch, ps=ps: e.copy(out=qkpre[:, ch, 3:3 + n], in_=ps[:, 0:n]), rd=[bp], wr=[b_qkpre])

    def front_v(l, nch, wv, bwv):
        for c in range(nch):
            ps, bp = nps()
            mm_tm(ps, bp, wv, bwv, 0, 512, c * 128, 512)
            kb.op(ACT, lambda e, c=c, ps=ps: e.copy(out=v_aug[:, c, :, 0:128],
                                                    in_=ps[:, 0:512].rearrange("p (h e) -> p h e", h=4)),
                  rd=[bp], wr=[b_vaug])

    def phaseA(l, xsrc, bsrc_list, halo_sb, b_halo):
        kb.op(POOL, lambda e: e.memset(Cst[:], 0.0), wr=[b_Cst])
        kb.op(POOL, lambda e: e.memset(Btot[:], 0.0), wr=[b_Btot])
        kb.op(POOL, lambda e: e.memset(v_aug[:, :, :, 128:129], 1.0), wr=[b_vaug])
        prenorm(halo_sb, b_halo, l, 0, HAL)
        w, bw = wload(l, "qk")
        front_k(l, HAL, w, bw)
        for ch in (2, 3):
            kb.op(POOL, lambda e, ch=ch: e.tensor_copy(out=qkpre[:, ch, 0:3], in_=qkpre[:, ch, HAL:HAL + 3]), wr=[b_qkpre])
        for i in range(NT):
            xt, xb = load_x(xsrc, bsrc_list[i], i, 0)
            prenorm(xt, xb, l, 0, T)
            w, bw = wload(l, "qk")
            front_k(l, T, w, bw)
            qk_conv(l, (2, 3), T)
            w, bw = wload(l, "v")
            front_v(l, 4, w, bw)
            w, bw = wload(l, "gt")
            gates(l, w, bw, 4)
            for c in range(4):
                state_update(c)

    def rope_chunk(ps, bp, out_ap, bout, n, rt, brt, halves=None):
        kb.op(ACT, lambda e: e.copy(out=a_bf[:, 0:n], in_=ps[:, 0:n]), rd=[bp], wr=[b_abf])
        ps2, bp2 = nps()
        kb.op(PE, lambda e: e.matmul(ps2[:, 0:n], lhsT=pswap_bf[:], rhs=a_bf[:, 0:n], start=True, stop=True),
              rd=[b_pswap, b_abf], wr=[bp2])
        kb.op(DVE, lambda e: e.tensor_tensor(out=r1[:, 0:n], in0=ps[:, 0:n], in1=rt[:, 0, 0:n], op=ALU.mult),
              rd=[bp, brt], wr=[b_r1])
        kb.op(DVE, lambda e: e.tensor_tensor(out=r2[:, 0:n], in0=ps2[:, 0:n], in1=rt[:, 1, 0:n], op=ALU.mult),
              rd=[bp2, brt], wr=[b_r2])
        if halves is None:
            kb.op(POOL, lambda e: e.tensor_tensor(out=out_ap, in0=r1[:, 0:n], in1=r2[:, 0:n], op=ALU.add),
                  rd=[b_r1, b_r2], wr=[bout])
        else:
            for (oap, sl) in halves:
                kb.op(POOL, lambda e, oap=oap, sl=sl: e.tensor_tensor(out=oap, in0=r1[sl, 0:n], in1=r2[sl, 0:n], op=ALU.add),
                      rd=[b_r1, b_r2], wr=[bout])

    def load_rope(pos0, n, slot):
        rt, brt = rope_t[slot]
        kb.dma(SP, rt[:, :, 0:n], rope_in.rearrange("p (a t) -> p a t", a=2)[:, :, pos0:pos0 + n], rd=[b_in], wr=[brt], sb=brt)
        return rt, brt

    def front_akv(l, n, nch, rt, brt, kcol0, vblk0):
        w, bw = wload(l, "akv")
        ps, bp = nps()
        mm_fm(ps, bp, w, bw, 0, n, wstride=256)
        rope_chunk(ps, bp, None, b_akT, n, rt, brt,
                   halves=[(akz[0:64, 0, kcol0:kcol0 + n], slice(0, 64)), (akz[64:128, 1, kcol0:kcol0 + n], slice(64, 128))])
        for c in range(nch):
            ps, bp = nps()
            mm_tm(ps, bp, w, bw, 128, 128, c * 128, 256)
            kb.op(ACT, lambda e, c=c, ps=ps: e.copy(out=av_ext[:, vblk0 + c, :].rearrange("p (g d) -> p g d", g=2)[:, :, 0:64],
                                                    in_=ps[:, 0:128].rearrange("p (g d) -> p g d", g=2)),
                  rd=[bp], wr=[b_av])

    def mlstm_chunk(l, c):
        cs = slice(c * 128, (c + 1) * 128)
        psS, bS = nps()

        for h in range(4):
            sl = slice((h % 2) * 64, (h % 2) * 64 + 64)
            kb.op(POOL, lambda e, h=h, sl=sl: e.tensor_copy(out=qz[sl, h, :], in_=qkT[sl, h // 2, cs]), rd=[b_qkT], wr=[b_qz])

        def fS(e):
            for h in range(4):
                r = e.matmul(psS[:, h * 128:(h + 1) * 128], lhsT=qkT[:, 2 + h // 2, cs], rhs=qz[:, h, :],
                             start=True, stop=True)
            return r
        kb.op(PE, fS, rd=[b_qkT, b_qz], wr=[bS])
        kb.op(DVE, lambda e: e.tensor_copy(out=lfrep[:], in_=lf[:, c, :].unsqueeze(2).to_broadcast([128, 4, 128])),
              rd=[b_lf], wr=[b_lfrep])
        psB, bB = nps()

        def fB(e):
            for h in range(4):
                r = e.matmul(psB[:, h * 128:(h + 1) * 128], lhsT=lfrep[:, h, :], rhs=tri_f, start=True, stop=True)
            return r
        kb.op(PE, fB, rd=[b_lfrep, b_cst], wr=[bB])
        for h in range(4):
            kb.op(ACT, lambda e, h=h: e.activation(out=Dt[:, h * 128:(h + 1) * 128], in_=psB[:, h * 128:(h + 1) * 128],
                                                   func=AF.Exp, bias=gsm[:, 0, c * 4 + h:c * 4 + h + 1]),
                  rd=[bB, b_gsm], wr=[b_Dt])
        kb.op(ACT, lambda e: e.activation(out=eb[:], in_=psB[:], func=AF.Exp), rd=[bB], wr=[b_eb])
        kb.op(POOL, lambda e: e.tensor_tensor(out=Dt[:].rearrange("p (h t) -> p h t", h=4),
                                              in0=Dt[:].rearrange("p (h t) -> p h t", h=4),
                                              in1=tri_f.unsqueeze(1).to_broadcast([128, 4, 128]), op=ALU.mult),
              rd=[b_cst], wr=[b_Dt])
        kb.op(DVE, lambda e: e.tensor_tensor(out=SdT[:], in0=psS[:], in1=Dt[:], op=ALU.mult), rd=[bS, b_Dt], wr=[b_SdT])
        for h in range(4):
            sl = slice((h % 2) * 64, (h % 2) * 64 + 64)
            kb.op(DVE, lambda e, h=h, sl=sl: e.tensor_tensor(out=qp[sl, h, :], in0=qkT[sl, h // 2, cs],
                                                            in1=eb[sl, h * 128:(h + 1) * 128], op=ALU.mult),
                  rd=[b_qkT, b_eb], wr=[b_qp])
        kb.op(POOL, lambda e: e.memset(hsm[:, 3, 0:4], 0.0), wr=[b_hsm])
        for hp in range(2):
            psH, bH = nps()

            def fH(e, hp=hp, psH=psH):
                for j in range(2):
                    h = hp * 2 + j
                    sl = slice((h % 2) * 64, (h % 2) * 64 + 64)
                    e.matmul(psH[:, j * 129:(j + 1) * 129], lhsT=SdT[:, h * 128:(h + 1) * 128], rhs=v_aug[:, c, h, :],
                             start=True, stop=False)
                    r = e.matmul(psH[:, j * 129:(j + 1) * 129], lhsT=qp[:, h, :], rhs=Cbf[:, h // 2, :],
                                 start=False, stop=True)
                return r
            kb.op(PE, fH, rd=[b_SdT, b_vaug, b_qp, b_Cbf], wr=[bH])
            H3 = psH[:, 0:258].rearrange("p (j e) -> p j e", j=2)
            kb.op(ACT, lambda e, H3=H3, hp=hp: e.activation(out=hsm[:, 0, hp * 2:hp * 2 + 2], in_=H3[:, :, 128], func=AF.Abs),
                  rd=[bH], wr=[b_hsm])
            kb.op(DVE, lambda e, hp=hp: e.tensor_scalar(out=hsm[:, 1, hp * 2:hp * 2 + 2], in0=hsm[:, 0, hp * 2:hp * 2 + 2],
                                                        scalar1=1.0, scalar2=None, op0=ALU.max), rd=[], wr=[b_hsm])
            kb.op(DVE, lambda e, hp=hp: e.reciprocal(out=hsm[:, 2, hp * 2:hp * 2 + 2], in_=hsm[:, 1, hp * 2:hp * 2 + 2]),
                  rd=[], wr=[b_hsm])
            for j in range(2):
                h = hp * 2 + j
                kb.op(DVE, lambda e, h=h, j=j, H3=H3: e.tensor_scalar(out=hh[:, h, :], in0=H3[:, j, 0:128],
                                                                     scalar1=hsm[:, 2, h:h + 1], scalar2=None, op0=ALU.mult),
                      rd=[bH, b_hsm], wr=[b_hh])
                kb.op(ACT, lambda e, h=h: e.activation(out=hjunk[:], in_=hh[:, h, :], func=AF.Square,
                                                       accum_out=hsm[:, 3, h:h + 1]), rd=[b_hh], wr=[b_hjunk, b_hsm])
        kb.op(DVE, lambda e: e.tensor_scalar(out=hsm[:, 4, 0:4], in0=hsm[:, 3, 0:4], scalar1=1.0 / 128.0, scalar2=None,
                                             op0=ALU.mult), rd=[], wr=[b_hsm])
        kb.op(ACT, lambda e: e.activation(out=hsm[:, 5, 0:4], in_=hsm[:, 4, 0:4], func=AF.Sqrt, bias=EPS), rd=[], wr=[b_hsm])
        kb.op(DVE, lambda e: e.reciprocal(out=hsm[:, 6, 0:4], in_=hsm[:, 5, 0:4]), rd=[], wr=[b_hsm])
        kb.op(POOL, lambda e: e.tensor_tensor(out=gs[:], in0=sig_o[:], in1=prm[:, l * PL + P_GMH:l * PL + P_GMH + 512],
                                              op=ALU.mult), rd=[b_sigo, b_prm], wr=[b_gs])
        for h in range(4):
            kb.op(DVE, lambda e, h=h: e.scalar_tensor_tensor(out=ym[:, h * 128:(h + 1) * 128], in0=hh[:, h, :],
                                                            scalar=hsm[:, 6, h:h + 1], in1=gs[:, h * 128:(h + 1) * 128],
                                                            op0=ALU.mult, op1=ALU.mult), rd=[b_hh, b_hsm, b_gs], wr=[b_ym])
        pb, bpb = npb()

        def fT(e):
            for h in range(4):
                r = e.transpose(pb[:, h * 128:(h + 1) * 128], ym[:, h * 128:(h + 1) * 128], ident_bf[:])
            return r
        kb.op(PE, fT, rd=[b_ym, b_ident], wr=[bpb])
        kb.op(ACT, lambda e: e.copy(out=yT[:, 0:4, cs], in_=pb[:, 0:512].rearrange("p (h t) -> p h t", h=4)),
              rd=[bpb], wr=[b_yT])
        state_update(c)

    def swa_chunk(l, c, first):
        cs = slice(c * 128, (c + 1) * 128)
        for g in range(2):
            sl = slice(g * 64, g * 64 + 64)
            for kbk in range(2):
                ps, bp = nps()
                mbi = 0 if kbk == 1 else (2 if first else 1)

                def f(e, ps=ps, kbk=kbk, mbi=mbi, g=g):
                    e.matmul(ps[:, 0:512], lhsT=akz[:, g, (c + kbk) * 128:(c + kbk + 1) * 128], rhs=aqT[:, :, cs],
                             start=True, stop=False)
                    return e.matmul(ps[:, 0:512], lhsT=ident_bf[:], rhs=mb_bf[:, mbi, :], start=False, stop=True)
                kb.op(PE, f, rd=[b_akT, b_aqT, b_ident, b_mb], wr=[bp])
                pt, bpt = PT[g * 2 + kbk]
                kb.op(ACT, lambda e, pt=pt, ps=ps: e.activation(out=pt[:], in_=ps[:, 0:512], func=AF.Exp, scale=0.125),
                      rd=[bp], wr=[bpt])
            psO, bO = nps()

            def fO(e, g=g, psO=psO):
                for h4 in range(4):
                    for kbk in range(2):
                        r = e.matmul(psO[:, h4 * 65:(h4 + 1) * 65], lhsT=PT[g * 2 + kbk][0][:, h4 * 128:(h4 + 1) * 128],
                                     rhs=av_ext[:, c + kbk, g * 65:(g + 1) * 65], start=(kbk == 0), stop=(kbk == 1))
                return r
            kb.op(PE, fO, rd=[PT[g * 2][1], PT[g * 2 + 1][1], b_av], wr=[bO])
            O3 = psO[:, 0:260].rearrange("p (h e) -> p h e", h=4)
            kb.op(DVE, lambda e, O3=O3, g=g: e.tensor_tensor(out=hsm[:, 7, g * 4:g * 4 + 4], in0=O3[:, :, 64],
                                                            in1=esink[:, l * 8 + g * 4:l * 8 + g * 4 + 4], op=ALU.add),
                  rd=[bO, b_esink], wr=[b_hsm])
            kb.op(DVE, lambda e, g=g: e.reciprocal(out=hsm[:, 7, g * 4:g * 4 + 4], in_=hsm[:, 7, g * 4:g * 4 + 4]),
                  rd=[], wr=[b_hsm])
            for h4 in range(4):
                hd = g * 4 + h4
                kb.op(DVE, lambda e, h4=h4, hd=hd, O3=O3: e.tensor_scalar(out=ya[:, hd * 64:(hd + 1) * 64], in0=O3[:, h4, 0:64],
                                                                         scalar1=hsm[:, 7, hd:hd + 1], scalar2=None, op0=ALU.mult),
                      rd=[bO, b_hsm], wr=[b_ya])
        pb, bpb = npb()

        def fT(e):
            for j in range(4):
                r = e.transpose(pb[:, j * 128:(j + 1) * 128], ya[:, j * 128:(j + 1) * 128], ident_bf[:])
            return r
        kb.op(PE, fT, rd=[b_ya, b_ident], wr=[bpb])
        kb.op(ACT, lambda e: e.copy(out=yT[:, 4:8, cs], in_=pb[:, 0:512].rearrange("p (h t) -> p h t", h=4)),
              rd=[bpb], wr=[b_yT])

    def phaseC(l, xsrc, bsrc_list, xdst, bdst_list, halo_sb, b_halo):
        kb.op(POOL, lambda e: e.memset(v_aug[:, :, :, 128:129], 1.0), wr=[b_vaug])
        kb.op(POOL, lambda e: e.memset(av_ext[:, :, 64:65], 1.0), wr=[b_av])
        kb.op(POOL, lambda e: e.memset(av_ext[:, :, 129:130], 1.0), wr=[b_av])
        kb.op(POOL, lambda e: e.memset(akz[:], 0.0), wr=[b_akT])
        kb.op(POOL, lambda e: e.memset(qp[:], 0.0), wr=[b_qp])
        kb.op(POOL, lambda e: e.memset(qz[:], 0.0), wr=[b_qz])
        prenorm(halo_sb, b_halo, l, 0, HAL)
        w, bw = wload(l, "qk")
        for ch in range(4):
            ps, bp = nps()
            mm_fm(ps, bp, w, bw, ch * 128, HAL, wstride=512)
            kb.op(ACT, lambda e, ch=ch, ps=ps: e.copy(out=qkpre[:, ch, 0:3], in_=ps[:, HAL - 3:HAL]), rd=[bp], wr=[b_qkpre])
        rt, brt = load_rope(0, HAL, 0)
        front_akv(l, HAL, 1, rt, brt, 0, 0)
        import os
        KS = os.environ.get("KSTOP", "")
        if KS == "h":
            return None
        for i in range(NT):
            xt, xb = load_x(xsrc, bsrc_list[i], i, 0)
            prenorm(xt, xb, l, 0, T)
            rt, brt = load_rope(HAL + i * T, T, 0)
            w, bw = wload(l, "qk")
            for ch in range(4):
                ps, bp = nps()
                mm_fm(ps, bp, w, bw, ch * 128, T, wstride=512)
                kb.op(ACT, lambda e, ch=ch, ps=ps: e.copy(out=qkpre[:, ch, 3:3 + T], in_=ps[:, 0:T]), rd=[bp], wr=[b_qkpre])
            qk_conv(l, (0, 1, 2, 3), T)
            w, bw = wload(l, "v")
            front_v(l, 4, w, bw)
            w, bw = wload(l, "gt")
            gates(l, w, bw, 4)
            wo_, bwo_ = wload(l, "o")
            w, bw = wload(l, "aq")
            for ch in range(4):
                ps, bp = nps()
                mm_fm(ps, bp, w, bw, ch * 128, T, wstride=512)
                rope_chunk(ps, bp, aqT[:, ch, 0:T], b_aqT, T, rt, brt)
            front_akv(l, T, 4, rt, brt, HAL, 1)
            if KS == "f":
                return None
            for c in range(4):
                ps, bp = nps()
                mm_tm(ps, bp, wo_, bwo_, 0, 512, c * 128, 512)
                kb.op(ACT, lambda e, ps=ps: e.activation(out=sig_o[:], in_=ps[:, 0:512], func=AF.Tanh, scale=0.5),
                      rd=[bp], wr=[b_sigo])
                kb.op(DVE, lambda e: e.tensor_scalar(out=sig_o[:], in0=sig_o[:], scalar1=0.5, scalar2=0.5,
                                                     op0=ALU.mult, op1=ALU.add), rd=[], wr=[b_sigo])
                mlstm_chunk(l, c)
                if KS == "m":
                    return None
                swa_chunk(l, c, first=(i == 0 and c == 0))
                if KS == "s":
                    return None
            kb.op(POOL, lambda e: e.tensor_copy(out=akz[:, :, 0:HAL], in_=akz[:, :, T:T + HAL]), wr=[b_akT])
            kb.op(POOL, lambda e: e.tensor_copy(out=av_ext[:, 0, :], in_=av_ext[:, 4, :]), wr=[b_av])
            for nb in range(2):
                w, bw = wload(l, f"out{nb}")
                for m in range(4):
                    ps, bp = nps()
                    mm_fm(ps, bp, w, bw, m * 128, T, rhs=yT, brhs=b_yT, wstride=512)
                    kb.op(ACT, lambda e, m=m, nb=nb, ps=ps: e.copy(out=ybuf[:, nb * 4 + m, :], in_=ps[:, 0:T]), rd=[bp], wr=[b_ybuf])
            postnorm_add(xt, xb, l, 1, T)
            kb.dma(SP, xdst.rearrange("p (c t) -> p c t", c=8)[:, :, i * T:(i + 1) * T], xt[:], rd=[xb], wr=[bdst_list[i]], sb=xb)
            last = (xt, xb)
        return last

    def ffn_up(l, n, halo_only):
        for b in range(11):
            w, bw = wload(l, f"up{b}")
            for pr in range(2):
                j = b * 2 + pr
                pss = []
                for part in range(2):
                    ps, bp = nps()
                    mm_fm(ps, bp, w, bw, part * 256 + pr * 128, n, wstride=512)
                    pss.append((ps, bp))
                for part in range(2):
                    ps, bp = pss[part]
                    ch = j + part * 22
                    if halo_only:
                        kb.op(ACT, lambda e, ch=ch, ps=ps: e.copy(out=tails[:, ch, :], in_=ps[:, n - 2:n]), rd=[bp], wr=[b_tails])
                        continue
                    up, bup = upre[part]
                    ua, bua = uacc[part]
                    P = prmh if part == 0 else prm
                    wc = lambda tap, ch=ch, P=P: P[:, l * PL + P_FW + ch * 3 + tap:l * PL + P_FW + ch * 3 + tap + 1]
                    kb.op(POOL, lambda e, ch=ch, up=up: e.tensor_copy(out=up[:, 0:2], in_=tails[:, ch, :]), rd=[b_tails], wr=[bup])
                    kb.op(ACT, lambda e, up=up, ps=ps: e.copy(out=up[:, 2:2 + n], in_=ps[:, 0:n]), rd=[bp], wr=[bup])
                    kb.op(POOL, lambda e, ch=ch, up=up: e.tensor_copy(out=tails[:, ch, :], in_=up[:, n:n + 2]), rd=[bup], wr=[b_tails])
                    kb.op(DVE, lambda e, up=up, ua=ua, wc=wc: e.tensor_scalar(out=ua[:, 0:n], in0=up[:, 2:2 + n], scalar1=wc(2),
                                                                           scalar2=None, op0=ALU.mult), rd=[bup, b_prm, b_prmh], wr=[bua])
                    for tap in range(2):
                        kb.op(DVE, lambda e, tap=tap, up=up, ua=ua, wc=wc: e.scalar_tensor_tensor(
                            out=ua[:, 0:n], in0=up[:, tap:tap + n], scalar=wc(tap), in1=ua[:, 0:n], op0=ALU.mult, op1=ALU.add),
                            rd=[bup, b_prm, b_prmh], wr=[bua])
                if halo_only:
                    continue
                kb.op(ACT, lambda e: e.activation(out=utanh[:, 0:n], in_=uacc[0][0][:, 0:n], func=AF.Tanh), rd=[uacc[0][1]], wr=[b_utanh])
                kb.op(DVE, lambda e: e.scalar_tensor_tensor(out=uu[:, 0:n], in0=utanh[:, 0:n], scalar=1.0, in1=uacc[0][0][:, 0:n],
                                                            op0=ALU.add, op1=ALU.mult), rd=[b_utanh, uacc[0][1]], wr=[b_uu])
                kb.op(POOL, lambda e, j=j: e.tensor_tensor(out=g_t[:, j, 0:n], in0=uu[:, 0:n], in1=uacc[1][0][:, 0:n], op=ALU.mult),
                      rd=[b_uu, uacc[1][1]], wr=[b_g])

    def phaseD(l, xsrc, bsrc_list, xdst, bdst_list, halo_sb, b_halo):
        prenorm(halo_sb, b_halo, l, 2, HAL)
        ffn_up(l, HAL, True)
        for i in range(NT):
            xt, xb = load_x(xsrc, bsrc_list[i], i, 0)
            prenorm(xt, xb, l, 2, T)
            ffn_up(l, T, False)
            for m in range(8):
                w, bw = wload(l, f"dn{m}")
                ps, bp = nps()
                mm_fm(ps, bp, w, bw, 0, T, kcn=22, rhs=g_t, brhs=b_g, wstride=128)
                kb.op(ACT, lambda e, m=m, ps=ps: e.copy(out=ybuf[:, m, :], in_=ps[:, 0:T]), rd=[bp], wr=[b_ybuf])
            postnorm_add(xt, xb, l, 3, T)
            kb.dma(SP, xdst.rearrange("p (c t) -> p c t", c=8)[:, :, i * T:(i + 1) * T], xt[:], rd=[xb], wr=[bdst_list[i]], sb=xb)
            last = (xt, xb)
        return last

    kb.dma(SP, xhalo[:].rearrange("p c t -> p (c t)"), xh_in, wr=[b_xhalo], sb=b_xhalo)
    b_xin = [b_in] * NT
    if phase == "A":
        phaseA(0, xT_in, b_xin, xhalo, b_xhalo)
        store_state()
    elif phase == "C":
        combine_state()
        import os
        if os.environ.get("KSTOP") != "1":
            phaseC(0, xT_in, b_xin, xo_d, b_xo, xhalo, b_xhalo)
    else:
        phaseD(0, xT_in, b_xin, xo_d, b_xo, xhalo, b_xhalo)
    kb.finish(kb.allbufs)
    return nc


def _consts(core):
    c = np.zeros((128, NCST), np.float32)
    c[:, 0:128] = np.eye(128, dtype=np.float32)
    s = np.arange(128)[:, None]
    t = np.arange(128)[None, :]
    c[:, 128:256] = (s <= t).astype(np.float32)
    pw = np.zeros((128, 128), np.float32)
    for m in range(128):
        d = m % 64
        pw[(m - d) + ((d + 32) % 64), m] = 1.0
    c[:, 256:384] = pw
    cur = np.where(s <= t, 0.0, NEG).astype(np.float32)
    prev = np.where(s > t, 0.0, NEG).astype(np.float32)
    c[:, 384:896] = np.tile(cur, (1, 4))
    c[:, 896:1408] = np.tile(prev, (1, 4))
    first = (core % 4 == 0)
    c[:, 1408:1920] = NEG if first else np.tile(prev, (1, 4))
    for r in range(NCORES):
        c[:, 1920 + r] = 1.0 if (r == core - 1 and not first) else 0.0
        c[:, 1928 + r] = 1.0 if (r // 4 == core // 4 and r < core) else 0.0
    return c


def _rope(core):
    pos0 = (core % 4) * SEG - HAL
    pos = (np.arange(HAL + SEG) + pos0).astype(np.float32)
    inv = (1.0 / (np.float32(10000.0) ** (np.arange(0, 64, 2, dtype=np.float32) / np.float32(64)))).astype(np.float32)
    ang = (pos[:, None] * inv[None, :]).astype(np.float32)
    cos = np.cos(ang).astype(np.float32)
    sin = np.sin(ang).astype(np.float32)
    out = np.zeros((128, 2, HAL + SEG), np.float32)
    for p in range(128):
        d = p % 64
        out[p, 0] = cos[:, d % 32]
        out[p, 1] = -sin[:, d % 32] if d < 32 else sin[:, d % 32]
    return out.reshape(128, -1)


def _params(inp, l):
    P = np.zeros((128, PL), np.float32)
    o = 0
    for n, key in enumerate(["g_pre_mix", "g_post_mix", "g_pre_ffn", "g_post_ffn"]):
        P[:, o + P_G + n * 8:o + P_G + n * 8 + 8] = inp[key][l].reshape(8, 128).T
    cw = inp["qk_conv_w"][l]
    P[:, o + P_QW:o + P_QW + 16] = cw.reshape(4, 4, 128).transpose(2, 1, 0).reshape(128, 16)
    P[:, o + P_QB:o + P_QB + 4] = inp["qk_conv_b"][l].reshape(4, 128).T
    fw = inp["ffn_conv_w"][l]
    P[:, o + P_FW:o + P_FW + 132] = fw.reshape(3, 44, 128).transpose(2, 1, 0).reshape(128, 132)
    P[:, o + P_GMH:o + P_GMH + 512] = inp["mh_norm_g"][l][None, :]
    P[:, o + P_GB:o + P_GB + 8] = inp["gate_bias"][l][None, :]
    P[:, o + P_SK:o + P_SK + 8] = inp["attn_sinks"][l][None, :]
    return P


def _to_fm(a):
    n = a.shape[0]
    return np.ascontiguousarray(a.T.reshape(8, 128, n).transpose(1, 0, 2).reshape(128, 8 * n))


def _from_fm(a, n):
    return a.reshape(128, 8, n).transpose(1, 0, 2).reshape(1024, n).T


_NC = {}


def _prog(phase):
    if phase not in _NC:
        _NC[phase] = build(phase)
    return _NC[phase]


def _halo(xfm_list, c):
    if c % 4 == 0:
        return np.zeros((128, 8 * HAL), np.float32)
    return np.ascontiguousarray(xfm_list[c - 1].reshape(128, 8, SEG)[:, :, SEG - HAL:].reshape(128, 8 * HAL))


def run_layers(inp, cores=None, nl=L, trace=None):
    inp = {k: np.asarray(v, dtype=np.float32) for k, v in inp.items()}
    cores = list(range(NCORES)) if cores is None else cores
    x = inp["x"]
    csts = {c: _consts(c) for c in cores}
    ropes = {c: _rope(c) for c in cores}
    xfm = {c: _to_fm(x[c // 4, (c % 4) * SEG:(c % 4 + 1) * SEG]) for c in cores}
    xfm_list = lambda d: [d.get(c) for c in range(NCORES)]
    for l in range(nl):
        prm = _params(inp, l)
        base = {c: {"xT": xfm[c], "xh": _halo(xfm_list(xfm), c), "prm": prm, "cst": csts[c]} for c in cores}
        resA = run_bass_kernel_spmd(_prog("A"), [dict(base[c], w_in=inp["w_in"][l]) for c in cores], core_ids=list(range(len(cores))))
        sg = np.zeros((NCORES * 128, 2 * 129 + 4), np.float32)
        for i, c in enumerate(cores):
            sg[c * 128:(c + 1) * 128] = np.asarray(resA.results[i]["spk"])
        resC = run_bass_kernel_spmd(_prog("C"), [dict(base[c], w_in=inp["w_in"][l], w_out=inp["w_out"][l], rope=ropes[c], sgall=sg)
                                                 for c in cores], core_ids=list(range(len(cores))))
        xm = {c: np.asarray(resC.results[i]["xo"]) for i, c in enumerate(cores)}
        if trace is not None:
            trace[f"xm{l}"] = xm
        resD = run_bass_kernel_spmd(_prog("D"), [{"xT": xm[c], "xh": _halo(xfm_list(xm), c), "prm": prm, "cst": csts[c],
                                                  "w_up": inp["w_up"][l], "w_down": inp["w_down"][l]} for c in cores],
                                    core_ids=list(range(len(cores))))
        xfm = {c: np.asarray(resD.results[i]["xo"]) for i, c in enumerate(cores)}
        if trace is not None:
            trace[f"x1{l}"] = xfm
    return xfm


def kernel(**inputs):
    xfm = run_layers(inputs)
    out = np.zeros((2, 4 * SEG, D), np.float32)
    for c in range(NCORES):
        out[c // 4, (c % 4) * SEG:(c % 4 + 1) * SEG] = _from_fm(xfm[c], SEG)
    return out
```
